# Optimizing a Trainium2 kernel written in Bass

```python
import math
import jax, jax.numpy as jnp
from jax import lax
import numpy as np

D_MODEL = 2048
BATCH = 2
SEQ = 16384
DEPTH = 1
DEC_BATCH = 4
DEC_SEQ = 4096
PAST_LEN = 128

N_Q_HEADS = 16
N_KV_HEADS = 4
HEAD_DIM = D_MODEL // N_Q_HEADS
Q_GROUP = N_Q_HEADS // N_KV_HEADS
D_ATTN = N_Q_HEADS * HEAD_DIM
D_KV = N_KV_HEADS * HEAD_DIM
WINDOW = 128
BLOCK = 128
NB_SIDE = -(-WINDOW // BLOCK)
KEY_BLOCK = (2 * NB_SIDE + 1) * BLOCK
D_SSM = D_MODEL // 2
SSM_GROUP = 16
N_SSM_GROUPS = D_SSM // SSM_GROUP
SSM_STATE = 64
N_DIR = 2
N_EXPERTS = 16
EC_CAPACITY_FACTOR = 2
D_EXPERT = D_MODEL
D_IN = D_ATTN + 2 * D_KV + D_SSM + 2 * D_MODEL
SPLITS = [D_ATTN, D_ATTN + D_KV, D_ATTN + 2 * D_KV, D_ATTN + 2 * D_KV + D_SSM,
          D_ATTN + 2 * D_KV + D_SSM + D_MODEL]
EPS = 1e-6
NEG_INF = -1e30

kernel_name = "hybrid_bidir_swa_s5_expert_choice_encoder"


def rms_norm(x, g):
    xf = x.astype(jnp.float32)
    y = xf * lax.rsqrt(jnp.mean(xf * xf, axis=-1, keepdims=True) + EPS) * g.astype(jnp.float32)
    return y.astype(x.dtype)


def alibi_slopes():
    h = jnp.arange(1, N_Q_HEADS + 1, dtype=jnp.float32)
    return jnp.exp2(-8.0 * h / N_Q_HEADS)


def windowed_gqa(q, k, v, sink, g_q, g_k):
    b, s = q.shape[0], q.shape[1]
    nb = s // BLOCK
    qf = rms_norm(q, g_q).astype(jnp.float32) * (HEAD_DIM ** -0.5)
    kf = rms_norm(k, g_k).astype(jnp.float32)
    vf = v.astype(jnp.float32)
    pad = ((0, 0), (NB_SIDE * BLOCK, NB_SIDE * BLOCK), (0, 0), (0, 0))
    kp = jnp.pad(kf, pad).reshape(b, nb + 2 * NB_SIDE, BLOCK, N_KV_HEADS, HEAD_DIM)
    vp = jnp.pad(vf, pad).reshape(b, nb + 2 * NB_SIDE, BLOCK, N_KV_HEADS, HEAD_DIM)
    kb = jnp.concatenate([kp[:, o:o + nb] for o in range(2 * NB_SIDE + 1)], axis=2)
    vb = jnp.concatenate([vp[:, o:o + nb] for o in range(2 * NB_SIDE + 1)], axis=2)
    qb = qf.reshape(b, nb, BLOCK, N_KV_HEADS, Q_GROUP, HEAD_DIM)
    scores = jnp.einsum('bnikgd,bnjkd->bnkgij', qb, kb)
    i = jnp.arange(BLOCK)[:, None]
    j = jnp.arange(KEY_BLOCK)[None, :]
    dist = jnp.abs(i - j + NB_SIDE * BLOCK)
    kpos = (jnp.arange(nb)[:, None] - NB_SIDE) * BLOCK + jnp.arange(KEY_BLOCK)[None, :]
    valid = (dist <= WINDOW)[None] & ((kpos >= 0) & (kpos < s))[:, None, :]
    slopes = alibi_slopes().reshape(N_KV_HEADS, Q_GROUP)
    scores = scores - slopes[None, None, :, :, None, None] * dist.astype(jnp.float32)
    scores = jnp.where(valid[None, :, None, None], scores, NEG_INF)
    sink_b = sink.astype(jnp.float32).reshape(N_KV_HEADS, Q_GROUP)[None, None, :, :, None, None]
    m = jnp.maximum(jnp.max(scores, axis=-1, keepdims=True), sink_b)
    p = jnp.exp(scores - m)
    p = p / (jnp.sum(p, axis=-1, keepdims=True) + jnp.exp(sink_b - m))
    out = jnp.einsum('bnkgij,bnjkd->bnikgd', p, vb)
    return out.reshape(b, s, D_ATTN).astype(q.dtype)


def _linear_recurrence(e1, e2):
    a1, b1 = e1
    a2, b2 = e2
    return a1 * a2, a2 * b1 + b2


def bidir_s5(u, lam_re, lam_im, log_step, b_re, b_im, c_re, c_im, d_skip):
    bsz, s = u.shape[0], u.shape[1]
    uf = u.astype(jnp.float32).reshape(bsz, s, N_SSM_GROUPS, SSM_GROUP)
    uc = uf.astype(jnp.complex64)
    y = d_skip.astype(jnp.float32).reshape(N_SSM_GROUPS, SSM_GROUP) * uf
    for direction in range(N_DIR):
        lam = lax.complex(lam_re[direction].astype(jnp.float32), lam_im[direction].astype(jnp.float32))
        step = jnp.exp(log_step[direction].astype(jnp.float32))[:, None]
        lam_bar = jnp.exp(lam * step)
        bmat = lax.complex(b_re[direction].astype(jnp.float32), b_im[direction].astype(jnp.float32))
        b_bar = ((lam_bar - 1.0) / lam)[..., None] * bmat
        bu = jnp.einsum('bsgh,gph->bsgp', uc, b_bar)
        a = jnp.broadcast_to(lam_bar, bu.shape)
        _, h = lax.associative_scan(_linear_recurrence, (a, bu), axis=1, reverse=(direction == 1))
        cmat = lax.complex(c_re[direction].astype(jnp.float32), c_im[direction].astype(jnp.float32))
        y = y + jnp.real(jnp.einsum('bsgp,ghp->bsgh', h, cmat))
    return y.reshape(bsz, s, D_SSM)


def expert_choice_ffn(x, w_router, w_gate, w_up, w_down):
    n_tok = x.shape[0] * x.shape[1]
    xt = x.reshape(n_tok, D_MODEL)
    aff = jax.nn.softmax(xt.astype(jnp.float32) @ w_router.astype(jnp.float32), axis=-1)
    cap = max(1, min(n_tok, EC_CAPACITY_FACTOR * n_tok // N_EXPERTS))
    gate, idx = lax.top_k(aff.T, cap)
    xe = xt[idx]
    h = jax.nn.silu(jnp.einsum('ecd,edf->ecf', xe, w_gate)) * jnp.einsum('ecd,edf->ecf', xe, w_up)
    ye = jnp.einsum('ecf,efd->ecd', h, w_down) * gate[..., None].astype(x.dtype)
    out = jnp.zeros_like(xt).at[idx.reshape(-1)].add(ye.reshape(-1, D_MODEL))
    return out.reshape(x.shape)


def encoder_layer(x, g_mix, w_in, g_q, g_k, attn_sink, lam_re, lam_im, log_step,
                  b_re, b_im, c_re, c_im, d_skip, w_glu, w_br_attn, w_br_ssm, w_out,
                  g_ffn, w_router, w_gate, w_up, w_down):
    b, s = x.shape[0], x.shape[1]
    xn = rms_norm(x, g_mix)
    proj = xn @ w_in
    q, k, v, u, gate_a, gate_s = jnp.split(proj, SPLITS, axis=-1)
    q = q.reshape(b, s, N_Q_HEADS, HEAD_DIM)
    k = k.reshape(b, s, N_KV_HEADS, HEAD_DIM)
    v = v.reshape(b, s, N_KV_HEADS, HEAD_DIM)
    attn = windowed_gqa(q, k, v, attn_sink, g_q, g_k)
    ssm = jax.nn.gelu(bidir_s5(u, lam_re, lam_im, log_step, b_re, b_im, c_re, c_im, d_skip)).astype(x.dtype)
    glu_val, glu_gate = jnp.split(ssm @ w_glu, 2, axis=-1)
    ssm_out = glu_val * jax.nn.sigmoid(glu_gate)
    merged = (jax.nn.sigmoid(gate_a) * (attn @ w_br_attn)
              + jax.nn.sigmoid(gate_s) * (ssm_out @ w_br_ssm))
    x = x + merged @ w_out
    x = x + expert_choice_ffn(rms_norm(x, g_ffn), w_router, w_gate, w_up, w_down)
    return x


def setup_inputs(seed: int = 0) -> dict:
    key = jax.random.key(seed)
    ks = jax.random.split(key, 24)
    f32 = jnp.float32
    L, G, P, H = DEPTH, N_SSM_GROUPS, SSM_STATE, SSM_GROUP
    nrm = lambda k, shape, scale: jax.random.normal(k, shape, f32) * scale
    lam_im_base = math.pi * jnp.arange(P, dtype=f32)
    return {
        "x_prompt": nrm(ks[0], (BATCH, SEQ, D_MODEL), 1.0),
        "x_sample": nrm(ks[1], (DEC_BATCH, DEC_SEQ, D_MODEL), 1.0),
        "g_mix": 1.0 + nrm(ks[2], (L, D_MODEL), 0.02),
        "w_in": nrm(ks[3], (L, D_MODEL, D_IN), D_MODEL ** -0.5),
        "g_q": 1.0 + nrm(ks[4], (L, HEAD_DIM), 0.02),
        "g_k": 1.0 + nrm(ks[5], (L, HEAD_DIM), 0.02),
        "attn_sink": nrm(ks[6], (L, N_Q_HEADS), 0.5),
        "lam_re": -0.5 + nrm(ks[7], (L, N_DIR, G, P), 0.01),
        "lam_im": lam_im_base + nrm(ks[8], (L, N_DIR, G, P), 0.01),
        "log_step": jax.random.uniform(ks[9], (L, N_DIR, G), f32, math.log(1e-3), math.log(1e-1)),
        "b_re": nrm(ks[10], (L, N_DIR, G, P, H), (2 * H) ** -0.5),
        "b_im": nrm(ks[11], (L, N_DIR, G, P, H), (2 * H) ** -0.5),
        "c_re": nrm(ks[12], (L, N_DIR, G, H, P), (2 * P) ** -0.5),
        "c_im": nrm(ks[13], (L, N_DIR, G, H, P), (2 * P) ** -0.5),
        "d_skip": 1.0 + nrm(ks[14], (L, D_SSM), 0.1),
        "w_glu": nrm(ks[15], (L, D_SSM, 2 * D_SSM), D_SSM ** -0.5),
        "w_br_attn": nrm(ks[16], (L, D_ATTN, D_MODEL), D_ATTN ** -0.5),
        "w_br_ssm": nrm(ks[17], (L, D_SSM, D_MODEL), D_SSM ** -0.5),
        "w_out": nrm(ks[18], (L, D_MODEL, D_MODEL), D_MODEL ** -0.5),
        "g_ffn": 1.0 + nrm(ks[19], (L, D_MODEL), 0.02),
        "w_router": nrm(ks[20], (L, D_MODEL, N_EXPERTS), D_MODEL ** -0.5),
        "w_gate": nrm(ks[21], (L, N_EXPERTS, D_MODEL, D_EXPERT), D_MODEL ** -0.5),
        "w_up": nrm(ks[22], (L, N_EXPERTS, D_MODEL, D_EXPERT), D_MODEL ** -0.5),
        "w_down": nrm(ks[23], (L, N_EXPERTS, D_EXPERT, D_MODEL), D_EXPERT ** -0.5),
    }


def reference(x_prompt, x_sample, g_mix, w_in, g_q, g_k, attn_sink, lam_re, lam_im, log_step,
              b_re, b_im, c_re, c_im, d_skip, w_glu, w_br_attn, w_br_ssm, w_out,
              g_ffn, w_router, w_gate, w_up, w_down):
    y_prompt = x_prompt
    y_sample = x_sample
    for l in range(DEPTH):
        layer_params = (g_mix[l], w_in[l], g_q[l], g_k[l], attn_sink[l], lam_re[l], lam_im[l],
                        log_step[l], b_re[l], b_im[l], c_re[l], c_im[l], d_skip[l], w_glu[l],
                        w_br_attn[l], w_br_ssm[l], w_out[l], g_ffn[l], w_router[l],
                        w_gate[l], w_up[l], w_down[l])
        y_prompt = encoder_layer(y_prompt, *layer_params)
        y_sample = encoder_layer(y_sample, *layer_params)
    return (y_prompt, y_sample)
```

```python
import math
from contextlib import ExitStack

import numpy as np
import concourse.bass as bass
import concourse.mybir as mybir
from concourse.bass_utils import run_bass_kernel_spmd

F32 = mybir.dt.float32
BF16 = mybir.dt.bfloat16
I32 = mybir.dt.int32
ALU = mybir.AluOpType
AF = mybir.ActivationFunctionType
AX = mybir.AxisListType

D = 2048
NH = 16
NKV = 4
HD = 128
DKV = 512
DSSM = 1024
NG = 64
PST = 64
HCH = 16
NE = 16
DIN = 8192
EPS = 1e-6
NEG = -1e30


class Tl:
    def __init__(self, t, name):
        self.t = t
        self.name = name
        self.w = []
        self.r = []
        self.gen_open = False

    def __getitem__(self, idx):
        return self.t[idx]


class K:
    def __init__(self, nc, es):
        self.nc = nc
        self.es = es
        self.engs = {"pe": nc.tensor, "act": nc.scalar, "dve": nc.vector, "pool": nc.gpsimd, "sp": nc.sync}
        self.sem = {}
        self.cnt = {}
        for e in self.engs:
            self.sem[e] = es.enter_context(nc.semaphore("c_" + e))
            self.cnt[e] = 0
        self.NDS = 6
        self.dsem = {}
        self.dcnt = {}
        for q in ("sp", "pool", "act"):
            self.dsem[q] = [es.enter_context(nc.semaphore("d_%s%d" % (q, i))) for i in range(self.NDS)]
            self.dcnt[q] = 0
        self.semobj = {}
        for e in self.engs:
            self.semobj[("c", e)] = self.sem[e]
        for q in self.dsem:
            for i, s in enumerate(self.dsem[q]):
                self.semobj[("d", q, i)] = s
        self.waited = {e: {} for e in self.engs}
        self.ntile = 0
        self.all_tokens = {}
        self.scopes = []

    def sb(self, name, shape, dt):
        self.ntile += 1
        t = self.es.enter_context(self.nc.sbuf_tensor("%s_%d" % (name, self.ntile), list(shape), dt))
        return Tl(t, name)

    def ps(self, name, shape, dt=F32):
        self.ntile += 1
        t = self.es.enter_context(self.nc.psum_tensor("%s_%d" % (name, self.ntile), list(shape), dt))
        return Tl(t, name)

    def dram(self, name, shape, dt):
        t = self.nc.dram_tensor(name, list(shape), dt).ap()
        return Tl(t, name)

    def wrap(self, ap, name):
        return Tl(ap, name)

    def _wait(self, eng, tok):
        key, val = tok
        if self.waited[eng].get(key, 0) >= val:
            return
        self.engs[eng].wait_ge(self.semobj[key], val)
        self.waited[eng][key] = val

    def _pre(self, eng, reads, writes, partial):
        for t in reads:
            for tok in t.w:
                if eng == "pe" and tok[0] == ("c", "pe"):
                    continue
                self._wait(eng, tok)
        for t in writes:
            if not (partial and t.gen_open):
                for tok in t.w:
                    if eng == "pe" and tok[0] == ("c", "pe"):
                        continue
                    self._wait(eng, tok)
            for tok in t.r:
                self._wait(eng, tok)

    def _post(self, tok, reads, writes, partial):
        key = tok[0]
        self.all_tokens[key] = tok[1]
        for t in reads:
            t.gen_open = False
            t.r = [x for x in t.r if x[0] != key] + [tok]
        for t in writes:
            if partial and t.gen_open:
                t.w = [x for x in t.w if x[0] != key] + [tok]
            else:
                t.w = [tok]
            t.r = []
            t.gen_open = bool(partial)

    def op(self, eng, fn, reads=(), writes=(), partial=False):
        self._pre(eng, reads, writes, partial)
        ins = fn()
        self.cnt[eng] += 1
        ins.then_inc(self.sem[eng], 1)
        self._post((("c", eng), self.cnt[eng]), reads, writes, partial)
        return ins

    def _dma_common(self, q, issue, reads, writes, partial):
        i = self.dcnt[q] % self.NDS
        rnd = self.dcnt[q] // self.NDS
        key = ("d", q, i)
        if rnd > 0:
            self._wait(q, (key, 16 * rnd))
        self._pre(q, reads, writes, partial)
        ins = issue()
        ins.then_inc(self.dsem[q][i], 16)
        self.dcnt[q] += 1
        self._post((key, 16 * (rnd + 1)), reads, writes, partial)
        return ins

    def dma(self, q, out, in_, reads=(), writes=(), partial=False, **kw):
        return self._dma_common(q, lambda: self.engs[q].dma_start(out=out, in_=in_, **kw), reads, writes, partial)

    def idma(self, out, out_off, in_, in_off, reads=(), writes=(), partial=True, **kw):
        return self._dma_common(
            "pool",
            lambda: self.nc.gpsimd.indirect_dma_start(out=out, out_offset=out_off, in_=in_, in_offset=in_off, **kw),
            reads, writes, partial)

    def barrier(self, engs=None):
        for e in (engs or list(self.engs)):
            for key, val in list(self.all_tokens.items()):
                self._wait(e, (key, val))

    def settle(self, tiles):
        for t in tiles:
            t.w = []
            t.r = []
            t.gen_open = False

    def scope(self):
        return _Scope(self)

    def finish(self):
        self.barrier()
        for sc in list(reversed(self.scopes)):
            sc.__exit__(None, None, None)

    def stage(self, n, skip):
        if n in skip:
            return
        with _Scope(self):
            yield n


class _Scope:
    def __init__(self, k):
        self.k = k
        self.open = False

    def __enter__(self):
        self.old = self.k.es
        self.st = ExitStack()
        self.st.__enter__()
        self.k.es = self.st
        self.open = True
        self.k.scopes.append(self)
        return self

    def __exit__(self, *a):
        if not self.open:
            return False
        self.open = False
        self.k.scopes.remove(self)
        self.k.es = self.old
        self.st.__exit__(None, None, None)
        return False


def make_cfg(ntc, seg, cap):
    assert ntc % 512 == 0 and seg % 512 == 0 and ntc % seg == 0 and cap % 128 == 0
    return dict(NTC=ntc, SEG=seg, NSEG=ntc // seg, NT=ntc // 128, NB=ntc // 512, CAP=cap)


FULL_CFG = make_cfg(32768, 4096, 4096)


def alibi_slopes():
    h = np.arange(1, NH + 1, dtype=np.float32)
    return np.exp2(-8.0 * h / NH).astype(np.float32)


def host_constants(cfg, seq_lens, cap_k):
    NTC, SEG, NSEG, NT, NB = cfg["NTC"], cfg["SEG"], cfg["NSEG"], cfg["NT"], cfg["NB"]
    c = {}
    starts = set()
    ends = set()
    pos = 0
    for L in seq_lens:
        starts.add(pos)
        pos += L
        ends.add(pos)
    nreal = pos
    p = nreal
    while p < NTC:
        starts.add(p)
        p += SEG
        ends.add(min(p, NTC))
    ends.add(NTC)
    kf = np.ones((128, NB), np.float32)
    kb = np.ones((128, NB), np.float32)
    for b in range(NB):
        if (b * 512) in starts:
            kf[:, b] = 0.0
        if ((b + 1) * 512) in ends:
            kb[:, b] = 0.0
    c["keepf"] = kf
    c["keepb"] = kb
    vb = np.zeros((128, NT, 3), np.float32)
    for n in range(NT):
        if (n * 128) in starts:
            vb[:, n, 0] = NEG
        if ((n + 1) * 128) in ends:
            vb[:, n, 2] = NEG
    c["vb"] = vb.reshape(128, NT * 3)
    sl = alibi_slopes().reshape(NKV, 4)
    kk = np.arange(128)[:, None]
    qq = np.arange(128)[None, :]
    tab = np.zeros((NKV, 128, 3, 4, 128), np.float32)
    for o in range(3):
        dist = np.abs(qq - kk - (o - 1) * 128).astype(np.float32)
        ok = dist <= 128
        for kv in range(NKV):
            for h in range(4):
                tab[kv, :, o, h, :] = np.where(ok, -sl[kv, h] * dist, NEG)
    c["atab"] = tab.reshape(NKV * 128, 3 * 4 * 128)
    valid = np.zeros((NTC,), np.float32)
    valid[:nreal] = 1.0
    c["valid"] = np.ascontiguousarray(valid.reshape(NT, 128).T)
    c["kcap"] = np.full((128, NE), float(cap_k), np.float32)
    c["ident"] = np.eye(128, dtype=np.float32)
    perm = np.zeros((128, 128), np.float32)
    for q in range(64):
        perm[q, 64 + q] = -1.0
        perm[64 + q, q] = 1.0
    c["perm"] = perm
    c["tri"] = np.triu(np.ones((128, 128), np.float32), 1)
    c["iota"] = np.tile(np.arange(512, dtype=np.float32)[None, :], (128, 1))
    tid = (np.arange(NT)[None, :] * 128 + np.arange(128)[:, None]).astype(np.float32)
    c["tokid"] = tid
    sgn = np.ones((128, 1), np.float32)
    sgn[:64] = -1.0
    c["sgn"] = sgn
    R_ = NE * cfg["CAP"] // 128
    c["slotid"] = (float(cfg["NTC"] - 512) + 2.0 * ((np.arange(128)[:, None] * R_ + np.arange(R_)[None, :]) % 128)).astype(np.float32)
    return c


CONST_SHAPES = lambda cfg: {
    "keepf": [128, cfg["NB"]], "keepb": [128, cfg["NB"]], "vb": [128, cfg["NT"] * 3],
    "atab": [NKV * 128, 3 * 4 * 128], "valid": [128, cfg["NT"]], "kcap": [128, NE],
    "ident": [128, 128], "perm": [128, 128], "tri": [128, 128], "iota": [128, 512],
    "tokid": [128, cfg["NT"]], "sgn": [128, 1], "slotid": [128, NE * cfg["CAP"] // 128],
}

PARAM_SHAPES = {
    "g_mix": [1, D], "w_in": [D, DIN], "g_q": [1, HD], "g_k": [1, HD], "attn_sink": [1, NH],
    "lam_re": [2, NG, PST], "lam_im": [2, NG, PST], "log_step": [2, NG],
    "b_re": [2, NG, PST, HCH], "b_im": [2, NG, PST, HCH], "c_re": [2, NG, HCH, PST], "c_im": [2, NG, HCH, PST],
    "d_skip": [1, DSSM], "w_glu": [DSSM, 2 * DSSM], "w_br_attn": [D, D], "w_br_ssm": [DSSM, D], "w_out": [D, D],
    "g_ffn": [1, D], "w_router": [D, NE], "w_gate": [NE, D, D], "w_up": [NE, D, D], "w_down": [NE, D, D],
}


class _Done(Exception):
    pass


def build_program(cfg, debug=False, stages=99, skip=(), cut=None):
    holder = {}
    try:
        return _build_program(cfg, debug, stages, skip, cut, holder)
    except _Done:
        return holder["nc"]


def _build_program(cfg, debug, stages, skip, cut, holder):
    NTC, SEG, NSEG, NT, NB, CAP = cfg["NTC"], cfg["SEG"], cfg["NSEG"], cfg["NT"], cfg["NB"], cfg["CAP"]
    nc = bass.Bass("TRN2", target_bir_lowering=False)
    holder["nc"] = nc
    P = {}
    for n, shp in PARAM_SHAPES.items():
        if stages < 6 and n in ("w_gate", "w_up", "w_down"):
            continue
        P[n] = nc.dram_tensor(n, shp, F32, kind="ExternalInput").ap()
    C = {}
    for n, shp in CONST_SHAPES(cfg).items():
        C[n] = nc.dram_tensor(n, shp, F32, kind="ExternalInput").ap()
    x_in = nc.dram_tensor("x", [NTC, D], F32, kind="ExternalInput").ap()
    y_out = nc.dram_tensor("y", [NTC, D], F32, kind="ExternalOutput").ap()
    skind = "ExternalOutput" if debug else "Internal"

    def scratch(name, shape, dt):
        return nc.dram_tensor(name, list(shape), dt, kind=skind).ap()

    xnT_d = scratch("xnT_d", [D, NTC], BF16)
    qT_d = scratch("qT_d", [D, NTC], BF16)
    kT_d = scratch("kT_d", [DKV, NTC + 256], BF16)
    v_d = scratch("v_d", [NTC + 256, DKV], BF16)
    uT_d = scratch("uT_d", [DSSM, NTC], BF16)
    gaT_d = scratch("gaT_d", [D, NTC], BF16)
    gsT_d = scratch("gsT_d", [D, NTC], BF16)
    attnT_d = scratch("attnT_d", [D, NTC], BF16)
    yf_d = scratch("yf_d", [DSSM, NTC], F32)
    yb_d = scratch("yb_d", [DSSM, NTC], F32)
    xn2_d = scratch("xn2_d", [NTC, D], BF16)
    aff_d = scratch("aff_d", [128, NE * NT], F32)
    lst_d = scratch("lst_d", [NE * CAP, 2], F32)

    with ExitStack() as es:
        k = K(nc, es)

        occ = {}

        def chk(n):
            if cut is not None and cut % 100 == n:
                occ[n] = occ.get(n, 0) + 1
                if occ[n] == cut // 100 + 1:
                    k.finish()
                    raise _Done()
        PS = [k.ps("ps%d" % i, [128, 512]) for i in range(8)]
        ident_f = k.sb("ident_f", [128, 128], F32)
        ident_b = k.sb("ident_b", [128, 128], BF16)
        ones_b = k.sb("ones_b", [128, 128], BF16)
        eps_t = k.sb("eps_t", [128, 1], F32)
        k.dma("sp", ident_f[:], C["ident"][:, :], writes=[ident_f])
        k.op("dve", lambda: nc.vector.tensor_copy(out=ident_b[:], in_=ident_f[:]), reads=[ident_f], writes=[ident_b])
        k.op("dve", lambda: nc.vector.memset(ones_b[:], 1.0), writes=[ones_b])
        k.op("dve", lambda: nc.vector.memset(eps_t[:], EPS), writes=[eps_t])

        for _sc in k.stage(1, skip):
            gbc = k.sb("gbc", [128, D], F32)
            k.dma("sp", gbc[:], P["g_mix"][0:1, :].to_broadcast([128, D]), writes=[gbc])
            xts = [k.sb("xt", [128, D], F32) for _ in range(2)]
            junk = k.sb("junk", [128, D], BF16)
            xns = [k.sb("xn", [128, D], BF16) for _ in range(2)]
            xTs = [k.sb("xT", [128, 16, 128], BF16) for _ in range(2)]
            ssq = [k.sb("ssq", [128, 1], F32) for _ in range(2)]
            std = [k.sb("std", [128, 1], F32) for _ in range(2)]
            rstd = [k.sb("rstd", [128, 1], F32) for _ in range(2)]
            PB = [k.ps("pb%d" % i, [128, 1024], BF16) for i in range(0)]
            xnT_v = xnT_d.rearrange("(c p) t -> p c t", p=128)
            for i in range(NT):
                a = i % 2
                xt, xn, xT = xts[a], xns[a], xTs[a]
                k.dma("sp", xt[:], x_in[i * 128:(i + 1) * 128, :], writes=[xt])
                k.op("act", lambda: nc.scalar.activation(out=junk[:], in_=xt[:], func=AF.Square, accum_out=ssq[a][:]), reads=[xt], writes=[junk, ssq[a]])
                k.op("act", lambda: nc.scalar.activation(out=std[a][:], in_=ssq[a][:], func=AF.Sqrt, bias=eps_t[:], scale=1.0 / D), reads=[ssq[a], eps_t], writes=[std[a]])
                k.op("dve", lambda: nc.vector.reciprocal(out=rstd[a][:], in_=std[a][:]), reads=[std[a]], writes=[rstd[a]])
                k.op("dve", lambda: nc.vector.scalar_tensor_tensor(out=xn[:], in0=xt[:], scalar=rstd[a][:, 0:1], in1=gbc[:], op0=ALU.mult, op1=ALU.mult), reads=[xt, rstd[a], gbc], writes=[xn])
                for hlf in range(2):
                    pst = PS[(2 * i + hlf) % 4]
                    pv = pst.t[:].bitcast(BF16)
                    for j in range(8):
                        c = hlf * 8 + j
                        k.op("pe", lambda: nc.tensor.transpose(out=pv[:, j * 128:(j + 1) * 128], in_=xn[:, c * 128:(c + 1) * 128], identity=ident_b[:]),
                             reads=[xn, ident_b], writes=[pst], partial=(j > 0))
                    eng = "act" if hlf == 0 else "dve"
                    if eng == "act":
                        k.op("act", lambda: nc.scalar.copy(out=xT[:, hlf * 8:(hlf + 1) * 8, :], in_=pv[:, 0:1024].rearrange("p (c t) -> p c t", c=8)), reads=[pst], writes=[xT], partial=False)
                    else:
                        k.op("dve", lambda: nc.vector.tensor_copy(out=xT[:, hlf * 8:(hlf + 1) * 8, :], in_=pv[:, 0:1024].rearrange("p (c t) -> p c t", c=8)), reads=[pst], writes=[xT], partial=True)
                k.dma("sp", xnT_v[:, :, i * 128:(i + 1) * 128], xT[:], reads=[xT])
        k.barrier()
        if stages <= 1:
            return nc

        for _sc in k.stage(2, skip):
            CG = 1024
            wts = [k.sb("w", [128, 16, CG], BF16) for _ in range(2)]
            xbs = [k.sb("xb", [128, 16, 512], BF16) for _ in range(2)]
            outs = [k.sb("o", [128, 512], BF16) for _ in range(4)]
            sqs = [k.sb("sq", [128, 512], BF16) for _ in range(2)]
            rs = [k.sb("rs", [128, 512], F32) for _ in range(2)]
            gq = k.sb("gq", [128, 1], F32)
            gk = k.sb("gk", [128, 1], F32)
            zt = k.sb("zt", [128, 512], BF16)
            k.dma("sp", gq[:], P["g_q"].rearrange("o p -> p o"), writes=[gq])
            k.dma("sp", gk[:], P["g_k"].rearrange("o p -> p o"), writes=[gk])
            k.op("dve", lambda: nc.vector.tensor_scalar(out=gq[:], in0=gq[:], scalar1=float(HD) ** -0.5, scalar2=None, op0=ALU.mult), reads=[gq], writes=[gq])
            k.op("dve", lambda: nc.vector.memset(zt[:], 0.0), writes=[zt])
            k.dma("sp", kT_d.rearrange("(c p) t -> p c t", p=128)[:, :, 0:128], zt[:, 0:512].rearrange("p (c t) -> p c t", c=4), reads=[zt])
            k.dma("sp", kT_d.rearrange("(c p) t -> p c t", p=128)[:, :, NTC + 128:NTC + 256], zt[:, 0:512].rearrange("p (c t) -> p c t", c=4), reads=[zt])
            k.dma("sp", v_d[0:128, :], zt[:], reads=[zt])
            k.dma("sp", v_d[NTC + 128:NTC + 256, :], zt[:], reads=[zt])
            w_v = P["w_in"].rearrange("(c p) n -> p c n", p=128)
            xnT_v = xnT_d.rearrange("(c p) t -> p c t", p=128)
            oc = 0
            pc = 0
            for g in range(DIN // CG):
                wt = wts[g % 2]
                k.dma("pool", wt[:], w_v[:, :, g * CG:(g + 1) * CG], writes=[wt])
                for b in range(NB):
                    xb = xbs[b % 2]
                    k.dma("sp", xb[:], xnT_v[:, :, b * 512:(b + 1) * 512], writes=[xb])
                    if g == 2:
                        chunks = list(range(4))
                    else:
                        chunks = list(range(CG // 128))
                    for ch in chunks:
                        col = g * CG + ch * 128
                        pt = PS[pc % 4]
                        pc += 1
                        for kk in range(16):
                            k.op("pe", lambda: nc.tensor.matmul(pt[:], lhsT=wt[:, kk, ch * 128:(ch + 1) * 128], rhs=xb[:, kk, :], start=(kk == 0), stop=(kk == 15)),
                                 reads=[wt, xb], writes=[pt], partial=(kk > 0))
                        o = outs[oc % 4]
                        oc += 1
                        if col < 2048 + 512:
                            sq = sqs[oc % 2]
                            r = rs[oc % 2]
                            p2 = PS[4 + (oc % 2)]
                            gsc = gq if col < 2048 else gk
                            k.op("act", lambda: nc.scalar.activation(out=sq[:], in_=pt[:], func=AF.Square), reads=[pt], writes=[sq])
                            k.op("pe", lambda: nc.tensor.matmul(p2[:], lhsT=ones_b[:], rhs=sq[:], start=True, stop=True), reads=[ones_b, sq], writes=[p2])
                            k.op("act", lambda: nc.scalar.activation(out=r[:], in_=p2[:], func=AF.Sqrt, bias=eps_t[:], scale=1.0 / HD), reads=[p2, eps_t], writes=[r])
                            k.op("dve", lambda: nc.vector.reciprocal(out=r[:], in_=r[:]), reads=[r], writes=[r])
                            k.op("dve", lambda: nc.vector.scalar_tensor_tensor(out=o[:], in0=pt[:], scalar=gsc[:, 0:1], in1=r[:], op0=ALU.mult, op1=ALU.mult), reads=[pt, gsc, r], writes=[o])
                            if col < 2048:
                                dst = qT_d[col:col + 128, b * 512:(b + 1) * 512]
                            else:
                                dst = kT_d[col - 2048:col - 2048 + 128, 128 + b * 512:128 + (b + 1) * 512]
                        elif col < 4096:
                            k.op("act", lambda: nc.scalar.copy(out=o[:], in_=pt[:]), reads=[pt], writes=[o])
                            dst = uT_d[col - 3072:col - 3072 + 128, b * 512:(b + 1) * 512]
                        else:
                            k.op("act", lambda: nc.scalar.activation(out=o[:], in_=pt[:], func=AF.Sigmoid), reads=[pt], writes=[o])
                            if col < 6144:
                                dst = gaT_d[col - 4096:col - 4096 + 128, b * 512:(b + 1) * 512]
                            else:
                                dst = gsT_d[col - 6144:col - 6144 + 128, b * 512:(b + 1) * 512]
                        k.dma("sp", dst, o[:], reads=[o])
                    if g == 2:
                        for sub in range(4):
                            pt = PS[pc % 4]
                            pc += 1
                            for kk in range(16):
                                k.op("pe", lambda: nc.tensor.matmul(pt[:], lhsT=xb[:, kk, sub * 128:(sub + 1) * 128], rhs=wt[:, kk, 512:1024], start=(kk == 0), stop=(kk == 15)),
                                     reads=[wt, xb], writes=[pt], partial=(kk > 0))
                            o = outs[oc % 4]
                            oc += 1
                            k.op("dve", lambda: nc.vector.tensor_copy(out=o[:], in_=pt[:]), reads=[pt], writes=[o])
                            t0 = 128 + b * 512 + sub * 128
                            k.dma("sp", v_d[t0:t0 + 128, :], o[:], reads=[o])
        k.barrier()
        if stages <= 2:
            return nc

        for _sc in k.stage(3, skip):
            QS = min(1024, NTC)
            nqb = QS // 128
            vb_t = k.sb("vb", [128, NT * 3], F32)
            k.dma("sp", vb_t[:], C["vb"][:, :], writes=[vb_t])
            sink_t = k.sb("sink", [128, NH], F32)
            k.dma("sp", sink_t[:], P["attn_sink"][0:1, :].to_broadcast([128, NH]), writes=[sink_t])
            k.op("act", lambda: nc.scalar.activation(out=sink_t[:], in_=sink_t[:], func=AF.Exp), reads=[sink_t], writes=[sink_t])
            zer = k.sb("zer", [128, 128], F32)
            k.op("dve", lambda: nc.vector.memset(zer[:], 0.0), writes=[zer])
            atab = k.sb("atab", [128, 3, 512], F32)
            est = k.sb("est", [128, 512], F32)
            q4s = [k.sb("q4", [128, 4, QS], BF16) for _ in range(2)]
            kts = [k.sb("kt", [128, QS + 256], BF16) for _ in range(2)]
            vts = [k.sb("vt", [128, nqb + 2, 128], BF16) for _ in range(2)]
            aos = [k.sb("ao", [128, 4, QS], BF16) for _ in range(2)]
            scs = [k.sb("sc", [128, 512], F32) for _ in range(3)]
            pts = [k.sb("pt", [128, 512], BF16) for _ in range(6)]
            dens = [k.sb("den", [128, 512], F32) for _ in range(2)]
            qT_v = qT_d.rearrange("(h p) t -> p h t", p=128)
            aT_v = attnT_d.rearrange("(h p) t -> p h t", p=128)
            it = 0
            for kv in range(NKV):
                k.dma("sp", atab[:], C["atab"][kv * 128:(kv + 1) * 128, :].rearrange("p (o n) -> p o n", o=3), writes=[atab])
                for h in range(4):
                    k.op("dve", lambda: nc.vector.tensor_scalar(out=est[:, h * 128:(h + 1) * 128], in0=zer[:], scalar1=sink_t[:, 4 * kv + h:4 * kv + h + 1], scalar2=None, op0=ALU.add),
                         reads=[zer, sink_t], writes=[est], partial=(h > 0))
                for sbk in range(NTC // QS):
                    t0 = sbk * QS
                    q4, kt, vt, ao = q4s[it % 2], kts[it % 2], vts[it % 2], aos[it % 2]
                    it += 1
                    k.dma("sp", q4[:], qT_v[:, 4 * kv:4 * kv + 4, t0:t0 + QS], writes=[q4])
                    k.dma("sp", kt[:], kT_d[kv * 128:(kv + 1) * 128, t0:t0 + QS + 256], writes=[kt])
                    k.dma("sp", vt[:], v_d[t0:t0 + QS + 256, kv * 128:(kv + 1) * 128].rearrange("(b p) c -> p b c", p=128), writes=[vt])
                    for n in range(nqb):
                        nb = sbk * nqb + n
                        ptl = []
                        for o in range(3):
                            pS = PS[(n % 2) * 3 + o]
                            k.op("pe", lambda: nc.tensor.matmul(pS[:].rearrange("p (h q) -> p h q", h=4), lhsT=kt[:, (n + o) * 128:(n + o + 1) * 128], rhs=q4[:, :, n * 128:(n + 1) * 128], start=True, stop=True),
                                 reads=[kt, q4], writes=[pS])
                            sc = scs[o]
                            pt = pts[(n % 2) * 3 + o]
                            k.op("dve", lambda: nc.vector.tensor_tensor(out=sc[:], in0=pS[:], in1=atab[:, o, :], op=ALU.add), reads=[pS, atab], writes=[sc])
                            k.op("act", lambda: nc.scalar.activation(out=pt[:], in_=sc[:], func=AF.Exp, bias=vb_t[:, nb * 3 + o:nb * 3 + o + 1], scale=1.0), reads=[sc, vb_t], writes=[pt])
                            ptl.append(pt)
                        pA = PS[6]
                        pD = PS[7]
                        for h in range(4):
                            for o in range(3):
                                k.op("pe", lambda: nc.tensor.matmul(pA[:, h * 128:(h + 1) * 128], lhsT=vt[:, n + o, :], rhs=ptl[o][:, h * 128:(h + 1) * 128], start=(o == 0), stop=(o == 2)),
                                     reads=[vt, ptl[o]], writes=[pA], partial=not (h == 0 and o == 0))
                        for o in range(3):
                            k.op("pe", lambda: nc.tensor.matmul(pD[:], lhsT=ones_b[:], rhs=ptl[o][:], start=(o == 0), stop=(o == 2)),
                                 reads=[ones_b, ptl[o]], writes=[pD], partial=(o > 0))
                        den = dens[n % 2]
                        k.op("dve", lambda: nc.vector.tensor_tensor(out=den[:], in0=pD[:], in1=est[:], op=ALU.add), reads=[pD, est], writes=[den])
                        k.op("dve", lambda: nc.vector.reciprocal(out=den[:], in_=den[:]), reads=[den], writes=[den])
                        k.op("dve", lambda: nc.vector.tensor_tensor(out=ao[:, :, n * 128:(n + 1) * 128], in0=pA[:].rearrange("p (h q) -> p h q", h=4), in1=den[:].rearrange("p (h q) -> p h q", h=4), op=ALU.mult),
                             reads=[pA, den], writes=[ao], partial=(n > 0))
                    k.dma("sp", aT_v[:, 4 * kv:4 * kv + 4, t0:t0 + QS], ao[:], reads=[ao])
        k.barrier()
        if stages <= 3:
            return nc

        GD = 2 * NG
        TWO_PI = 2.0 * math.pi
        for _sc in k.stage(4, skip):
            perm_f = k.sb("perm_f", [128, 128], F32)
            sgn = k.sb("sgn", [128, 1], F32)
            nsgn = k.sb("nsgn", [128, 1], F32)
            iota = k.sb("iota", [128, 512], F32)
            keepf = k.sb("keepf", [128, NB], F32)
            keepb = k.sb("keepb", [128, NB], F32)
            k.dma("sp", perm_f[:], C["perm"][:, :], writes=[perm_f])
            k.dma("sp", sgn[:], C["sgn"][:, :], writes=[sgn])
            k.dma("sp", iota[:], C["iota"][:, :], writes=[iota])
            k.dma("sp", keepf[:], C["keepf"][:, :], writes=[keepf])
            k.dma("sp", keepb[:], C["keepb"][:, :], writes=[keepb])
            k.op("dve", lambda: nc.vector.tensor_scalar(out=nsgn[:], in0=sgn[:], scalar1=-1.0, scalar2=None, op0=ALU.mult), reads=[sgn], writes=[nsgn])

            def small(name, dt=F32):
                return k.sb(name, [128, GD], dt)

            LR, LI, LS = small("LR"), small("LI"), small("LS")
            for half in range(2):
                sl = slice(half * 64, half * 64 + 64)
                k.dma("sp", LR[sl, :], P["lam_re"].rearrange("d g p -> p (d g)"), writes=[LR], partial=(half > 0), allow_slow_non_contiguous=True)
                k.dma("sp", LI[sl, :], P["lam_im"].rearrange("d g p -> p (d g)"), writes=[LI], partial=(half > 0), allow_slow_non_contiguous=True)
            k.dma("sp", LS[:], P["log_step"].rearrange("(o d) g -> o (d g)", o=1).to_broadcast([128, GD]), writes=[LS])
            STEP, RHO, TF, KR, KIS = small("STEP"), small("RHO"), small("TF"), small("KR"), small("KIS")
            t_a, t_b, t_c, t_d = small("t_a"), small("t_b"), small("t_c"), small("t_d")
            t_i = small("t_i", I32)
            CR5, SR5 = small("CR5"), small("SR5")

            def V(fn, reads, writes):
                k.op("dve", fn, reads=reads, writes=writes)

            def frac_of(dst, src):
                V(lambda: nc.vector.tensor_copy(out=t_i[:], in_=src[:]), [src], [t_i])
                V(lambda: nc.vector.tensor_copy(out=t_d[:], in_=t_i[:]), [t_i], [t_d])
                V(lambda: nc.vector.tensor_tensor(out=dst[:], in0=src[:], in1=t_d[:], op=ALU.subtract), [src, t_d], [dst])

            def sincos_turns(sin_dst, cos_dst, turns):
                frac_of(t_a, turns)
                k.op("act", lambda: nc.scalar.activation(out=sin_dst[:], in_=t_a[:], func=AF.Sin, scale=TWO_PI), reads=[t_a], writes=[sin_dst])
                V(lambda: nc.vector.tensor_scalar(out=t_a[:], in0=turns[:], scalar1=0.25, scalar2=None, op0=ALU.add), [turns], [t_a])
                frac_of(t_a, t_a)
                k.op("act", lambda: nc.scalar.activation(out=cos_dst[:], in_=t_a[:], func=AF.Sin, scale=TWO_PI), reads=[t_a], writes=[cos_dst])

            k.op("act", lambda: nc.scalar.activation(out=STEP[:], in_=LS[:], func=AF.Exp), reads=[LS], writes=[STEP])
            V(lambda: nc.vector.tensor_tensor(out=t_b[:], in0=LR[:], in1=STEP[:], op=ALU.mult), [LR, STEP], [t_b])
            k.op("act", lambda: nc.scalar.activation(out=RHO[:], in_=t_b[:], func=AF.Exp), reads=[t_b], writes=[RHO])
            V(lambda: nc.vector.tensor_tensor(out=t_b[:], in0=LI[:], in1=STEP[:], op=ALU.mult), [LI, STEP], [t_b])
            V(lambda: nc.vector.tensor_scalar(out=t_b[:], in0=t_b[:], scalar1=1.0 / TWO_PI, scalar2=None, op0=ALU.mult), [t_b], [t_b])
            frac_of(TF, t_b)
            S1, C1 = small("S1"), small("C1")
            sincos_turns(S1, C1, TF)
            NR, NI, DEN = small("NR"), small("NI"), small("DEN")
            V(lambda: nc.vector.tensor_tensor(out=NR[:], in0=RHO[:], in1=C1[:], op=ALU.mult), [RHO, C1], [NR])
            V(lambda: nc.vector.tensor_scalar(out=NR[:], in0=NR[:], scalar1=-1.0, scalar2=None, op0=ALU.add), [NR], [NR])
            V(lambda: nc.vector.tensor_tensor(out=NI[:], in0=RHO[:], in1=S1[:], op=ALU.mult), [RHO, S1], [NI])
            V(lambda: nc.vector.tensor_tensor(out=DEN[:], in0=LR[:], in1=LR[:], op=ALU.mult), [LR], [DEN])
            V(lambda: nc.vector.tensor_tensor(out=t_b[:], in0=LI[:], in1=LI[:], op=ALU.mult), [LI], [t_b])
            V(lambda: nc.vector.tensor_tensor(out=DEN[:], in0=DEN[:], in1=t_b[:], op=ALU.add), [DEN, t_b], [DEN])
            V(lambda: nc.vector.reciprocal(out=DEN[:], in_=DEN[:]), [DEN], [DEN])
            V(lambda: nc.vector.tensor_tensor(out=KR[:], in0=NR[:], in1=LR[:], op=ALU.mult), [NR, LR], [KR])
            V(lambda: nc.vector.tensor_tensor(out=t_b[:], in0=NI[:], in1=LI[:], op=ALU.mult), [NI, LI], [t_b])
            V(lambda: nc.vector.tensor_tensor(out=KR[:], in0=KR[:], in1=t_b[:], op=ALU.add), [KR, t_b], [KR])
            V(lambda: nc.vector.tensor_tensor(out=KR[:], in0=KR[:], in1=DEN[:], op=ALU.mult), [KR, DEN], [KR])
            V(lambda: nc.vector.tensor_tensor(out=KIS[:], in0=NI[:], in1=LR[:], op=ALU.mult), [NI, LR], [KIS])
            V(lambda: nc.vector.tensor_tensor(out=t_b[:], in0=NR[:], in1=LI[:], op=ALU.mult), [NR, LI], [t_b])
            V(lambda: nc.vector.tensor_tensor(out=KIS[:], in0=KIS[:], in1=t_b[:], op=ALU.subtract), [KIS, t_b], [KIS])
            V(lambda: nc.vector.tensor_tensor(out=KIS[:], in0=KIS[:], in1=DEN[:], op=ALU.mult), [KIS, DEN], [KIS])
            V(lambda: nc.vector.tensor_scalar(out=KIS[:], in0=KIS[:], scalar1=sgn[:, 0:1], scalar2=None, op0=ALU.mult), [KIS, sgn], [KIS])
            V(lambda: nc.vector.tensor_scalar(out=t_c[:], in0=TF[:], scalar1=512.0, scalar2=None, op0=ALU.mult), [TF], [t_c])
            sincos_turns(SR5, CR5, t_c)
            NSR5 = small("NSR5")
            V(lambda: nc.vector.tensor_scalar(out=NSR5[:], in0=SR5[:], scalar1=-1.0, scalar2=None, op0=ALU.mult), [SR5], [NSR5])

            X1 = k.sb("X1", [128, GD, HCH], F32)
            X2 = k.sb("X2", [128, GD, HCH], F32)
            bre = P["b_re"].rearrange("d g p h -> p (d g) h")
            bim = P["b_im"].rearrange("d g p h -> p (d g) h")
            k.dma("sp", X1[0:64, :, :], bre, writes=[X1])
            k.dma("sp", X1[64:128, :, :], bim, writes=[X1], partial=True)
            k.dma("sp", X2[0:64, :, :], bim, writes=[X2])
            k.dma("sp", X2[64:128, :, :], bre, writes=[X2], partial=True)
            CN1 = k.sb("CN1", [HCH, 2, 8, 128], F32)
            CN2 = k.sb("CN2", [HCH, 2, 8, 128], F32)
            cre = P["c_re"].rearrange("d g h p -> h d g p")
            cim = P["c_im"].rearrange("d g h p -> h d g p")

            if stages == 3.1:
                k.finish()
                return nc
            uT = k.sb("uT", [128, NTC], BF16)
            Ag = [k.sb("Ag", [128, 128], F32) for _ in range(2)]
            t16 = [k.sb("t16", [128, HCH], F32) for _ in range(2)]
            LB = [k.sb("LB", [128, 128], BF16) for _ in range(2)]
            LBs = [k.sb("LBs", [128, 128], BF16) for _ in range(2)]
            W1 = [k.sb("W1", [128, HCH], BF16) for _ in range(2)]
            W2 = [k.sb("W2", [128, HCH], BF16) for _ in range(2)]
            ROT = [k.sb("ROT", [128, 128], F32) for _ in range(2)]
            rtmp = [k.sb("rtmp", [128, 128], F32) for _ in range(2)]
            CT = [k.sb("CT", [128, 512], F32) for _ in range(2)]
            ST = [k.sb("ST", [128, 512], F32) for _ in range(2)]
            xa = [k.sb("xa", [128, 512], F32) for _ in range(2)]
            xi = [k.sb("xi", [128, 512], I32) for _ in range(2)]
            xf = [k.sb("xf", [128, 512], F32) for _ in range(2)]
            T1 = [k.sb("T1", [128, 512], F32) for _ in range(2)]
            T2 = [k.sb("T2", [128, 512], F32) for _ in range(2)]
            BT = [k.sb("BT", [128, 512], F32) for _ in range(2)]
            GG = [[k.sb("GG", [128, 512], F32) for _ in range(2)] for _ in range(2)]
            D1 = [k.sb("D1", [128, 512], BF16) for _ in range(2)]
            D2 = [k.sb("D2", [128, 512], BF16) for _ in range(2)]
            INI = [k.sb("INI", [128, 1], F32) for _ in range(2)]
            YO = [[k.sb("YO", [HCH, 512], F32) for _ in range(2)] for _ in range(2)]
            PW = PS[7]
            for gs in range(NG // 8):
                k.dma("sp", uT[:], uT_d[gs * 128:(gs + 1) * 128, :], writes=[uT])
                for dd in range(2):
                    g0 = gs * 8
                    k.dma("sp", CN1[:, dd, :, 0:64], cre[:, dd, g0:g0 + 8, :], writes=[CN1], partial=(dd > 0))
                    k.dma("sp", CN1[:, dd, :, 64:128], cim[:, dd, g0:g0 + 8, :], writes=[CN1], partial=True)
                    k.dma("sp", CN2[:, dd, :, 0:64], cim[:, dd, g0:g0 + 8, :], writes=[CN2], partial=(dd > 0))
                    k.dma("sp", CN2[:, dd, :, 64:128], cre[:, dd, g0:g0 + 8, :], writes=[CN2], partial=True)
                for gl in range(8):
                    g = gs * 8 + gl
                    for d in range(2):
                        gd = d * NG + g
                        V(lambda: nc.vector.memset(Ag[d][:], 0.0), [], [Ag[d]])
                        V(lambda: nc.vector.tensor_scalar(out=t16[d][:], in0=X1[:, gd, :], scalar1=KR[:, gd:gd + 1], scalar2=None, op0=ALU.mult), [X1, KR], [t16[d]])
                        k.op("dve", lambda: nc.vector.scalar_tensor_tensor(out=Ag[d][:, gl * 16:(gl + 1) * 16], in0=X2[:, gd, :], scalar=KIS[:, gd:gd + 1], in1=t16[d][:], op0=ALU.mult, op1=ALU.add),
                             reads=[X2, KIS, t16[d]], writes=[Ag[d]], partial=True)
                        chk(1)
                        k.op("pe", lambda: nc.tensor.matmul(PW[:, 0:128], lhsT=Ag[d][:], rhs=ident_f[:], start=True, stop=True), reads=[Ag[d], ident_f], writes=[PW])
                        k.op("pe", lambda: nc.tensor.matmul(PW[:, 128:256], lhsT=Ag[d][:], rhs=perm_f[:], start=True, stop=True), reads=[Ag[d], perm_f], writes=[PW], partial=True)
                        chk(2)
                        k.op("pe", lambda: nc.tensor.transpose(out=PW[:, 256:256 + HCH], in_=CN1[:, d, gl, :], identity=ident_f[0:HCH, 0:HCH]), reads=[CN1, ident_f], writes=[PW], partial=True)
                        k.op("pe", lambda: nc.tensor.transpose(out=PW[:, 288:288 + HCH], in_=CN2[:, d, gl, :], identity=ident_f[0:HCH, 0:HCH]), reads=[CN2, ident_f], writes=[PW], partial=True)
                        chk(3)
                        k.op("act", lambda: nc.scalar.copy(out=LB[d][:], in_=PW[:, 0:128]), reads=[PW], writes=[LB[d]])
                        k.op("act", lambda: nc.scalar.copy(out=LBs[d][:], in_=PW[:, 128:256]), reads=[PW], writes=[LBs[d]])
                        chk(31)
                        k.op("act", lambda: nc.scalar.activation(out=W1[d][:], in_=PW[:, 256:256 + HCH], func=AF.Copy, scale=nsgn[:, 0:1]), reads=[PW, nsgn], writes=[W1[d]])
                        chk(32)
                        k.op("act", lambda: nc.scalar.activation(out=W2[d][:], in_=PW[:, 288:288 + HCH], func=AF.Copy, scale=-1.0), reads=[PW], writes=[W2[d]])
                        chk(4)
                        V(lambda: nc.vector.tensor_scalar(out=rtmp[d][:], in0=perm_f[:], scalar1=NSR5[:, gd:gd + 1], scalar2=None, op0=ALU.mult), [perm_f, NSR5], [rtmp[d]])
                        V(lambda: nc.vector.scalar_tensor_tensor(out=ROT[d][:], in0=ident_f[:], scalar=CR5[:, gd:gd + 1], in1=rtmp[d][:], op0=ALU.mult, op1=ALU.add), [ident_f, CR5, rtmp[d]], [ROT[d]])
                        chk(5)
                        V(lambda: nc.vector.tensor_scalar(out=xa[d][:], in0=iota[:], scalar1=TF[:, gd:gd + 1], scalar2=None, op0=ALU.mult), [iota, TF], [xa[d]])
                        V(lambda: nc.vector.tensor_copy(out=xi[d][:], in_=xa[d][:]), [xa[d]], [xi[d]])
                        V(lambda: nc.vector.tensor_copy(out=xf[d][:], in_=xi[d][:]), [xi[d]], [xf[d]])
                        V(lambda: nc.vector.tensor_tensor(out=xf[d][:], in0=xa[d][:], in1=xf[d][:], op=ALU.subtract), [xa[d], xf[d]], [xf[d]])
                        chk(6)
                        k.op("act", lambda: nc.scalar.activation(out=ST[d][:], in_=xf[d][:], func=AF.Sin, scale=TWO_PI), reads=[xf[d]], writes=[ST[d]])
                        chk(7)
                        V(lambda: nc.vector.tensor_scalar(out=xa[d][:], in0=xa[d][:], scalar1=0.25, scalar2=None, op0=ALU.add), [xa[d]], [xa[d]])
                        V(lambda: nc.vector.tensor_copy(out=xi[d][:], in_=xa[d][:]), [xa[d]], [xi[d]])
                        V(lambda: nc.vector.tensor_copy(out=xf[d][:], in_=xi[d][:]), [xi[d]], [xf[d]])
                        V(lambda: nc.vector.tensor_tensor(out=xf[d][:], in0=xa[d][:], in1=xf[d][:], op=ALU.subtract), [xa[d], xf[d]], [xf[d]])
                        chk(8)
                        k.op("act", lambda: nc.scalar.activation(out=CT[d][:], in_=xf[d][:], func=AF.Sin, scale=TWO_PI), reads=[xf[d]], writes=[CT[d]])
                        chk(9)
                    if stages == 3.2:
                        k.finish()
                        return nc
                    for stp in range(NB):
                        for d in range(2):
                            gd = d * NG + g
                            b = stp if d == 0 else NB - 1 - stp
                            keep = keepf if d == 0 else keepb
                            P1 = PS[d * 2]
                            P2 = PS[d * 2 + 1]
                            PY = PS[4 + d]
                            PR = PS[6]
                            ub = uT[:, b * 512:(b + 1) * 512]
                            k.op("pe", lambda: nc.tensor.matmul(P1[:], lhsT=LB[d][:], rhs=ub, start=True, stop=True), reads=[LB[d], uT], writes=[P1])
                            k.op("pe", lambda: nc.tensor.matmul(P2[:], lhsT=LBs[d][:], rhs=ub, start=True, stop=True), reads=[LBs[d], uT], writes=[P2])
                            p1v = P1.t[:, :] if d == 0 else P1.t[:, ::-1]
                            p2v = P2.t[:, :] if d == 0 else P2.t[:, ::-1]
                            V(lambda: nc.vector.tensor_tensor(out=T1[d][:], in0=p1v, in1=CT[d][:], op=ALU.mult), [P1, CT[d]], [T1[d]])
                            V(lambda: nc.vector.tensor_tensor(out=T2[d][:], in0=p2v, in1=ST[d][:], op=ALU.mult), [P2, ST[d]], [T2[d]])
                            k.op("pool", lambda: nc.gpsimd.tensor_tensor(out=BT[d][:], in0=T1[d][:], in1=T2[d][:], op=ALU.add), reads=[T1[d], T2[d]], writes=[BT[d]])
                            G = GG[d][stp % 2]
                            Gp = GG[d][(stp + 1) % 2]
                            if stp == 0:
                                V(lambda: nc.vector.tensor_tensor_scan(out=G[:], data0=RHO[:, gd:gd + 1].to_broadcast([128, 512]), data1=BT[d][:], initial=0.0, op0=ALU.mult, op1=ALU.add),
                                  [RHO, BT[d]], [G])
                            else:
                                k.op("pe", lambda: nc.tensor.matmul(PR[:, d:d + 1], lhsT=ROT[d][:], rhs=Gp[:, 511:512], start=True, stop=True), reads=[ROT[d], Gp], writes=[PR], partial=True)
                                V(lambda: nc.vector.tensor_scalar(out=INI[d][:], in0=PR[:, d:d + 1], scalar1=keep[:, b:b + 1], scalar2=None, op0=ALU.mult), [PR, keep], [INI[d]])
                                V(lambda: nc.vector.tensor_tensor_scan(out=G[:], data0=RHO[:, gd:gd + 1].to_broadcast([128, 512]), data1=BT[d][:], initial=INI[d][:, 0:1], op0=ALU.mult, op1=ALU.add),
                                  [RHO, BT[d], INI[d]], [G])
                            k.op("pool", lambda: nc.gpsimd.tensor_tensor(out=D1[d][:], in0=G[:], in1=CT[d][:], op=ALU.mult), reads=[G, CT[d]], writes=[D1[d]])
                            k.op("pool", lambda: nc.gpsimd.tensor_tensor(out=D2[d][:], in0=G[:], in1=ST[d][:], op=ALU.mult), reads=[G, ST[d]], writes=[D2[d]])
                            k.op("pe", lambda: nc.tensor.matmul(PY[0:HCH, :], lhsT=W1[d][:], rhs=D1[d][:], start=True, stop=False), reads=[W1[d], D1[d]], writes=[PY])
                            k.op("pe", lambda: nc.tensor.matmul(PY[0:HCH, :], lhsT=W2[d][:], rhs=D2[d][:], start=False, stop=True), reads=[W2[d], D2[d]], writes=[PY], partial=True)
                            yo = YO[d][stp % 2]
                            yov = yo.t[:, :] if d == 0 else yo.t[:, ::-1]
                            k.op("act", lambda: nc.scalar.copy(out=yov, in_=PY[0:HCH, :]), reads=[PY], writes=[yo])
                            ydst = yf_d if d == 0 else yb_d
                            k.dma("sp", ydst[g * 16:(g + 1) * 16, b * 512:(b + 1) * 512], yo[:], reads=[yo])
        k.barrier()
        if stages <= 4:
            return nc
        soT_d = scratch("soT_d", [DSSM, NTC], BF16)
        mT_d = scratch("mT_d", [D, NTC], BF16)

        for _sc in k.stage(5, skip):
            wg = k.sb("wglu", [128, 8, 2 * DSSM], BF16)
            k.dma("pool", wg[:], P["w_glu"].rearrange("(c p) n -> p c n", p=128), writes=[wg])
            dsk = k.sb("dsk", [128, 8], F32)
            k.dma("sp", dsk[:], P["d_skip"].rearrange("o (c p) -> p (o c)", p=128), writes=[dsk], allow_slow_non_contiguous=True)
            yfv = yf_d.rearrange("(c p) t -> p c t", p=128)
            ybv = yb_d.rearrange("(c p) t -> p c t", p=128)
            uTv = uT_d.rearrange("(c p) t -> p c t", p=128)
            soTv = soT_d.rearrange("(c p) t -> p c t", p=128)
            yft = k.sb("yft", [128, 8, 512], F32)
            ybt = k.sb("ybt", [128, 8, 512], F32)
            ut = k.sb("ut", [128, 8, 512], BF16)
            zt = k.sb("z", [128, 8, 512], F32)
            z2 = k.sb("z2", [128, 8, 512], F32)
            ssm = [k.sb("ssm", [128, 8, 512], BF16) for _ in range(2)]
            sgs = [k.sb("sg", [128, 512], F32) for _ in range(2)]
            sot = [k.sb("sot", [128, 8, 512], BF16) for _ in range(2)]
            for b in range(NB):
                bs = slice(b * 512, (b + 1) * 512)
                k.dma("sp", yft[:], yfv[:, :, bs], writes=[yft])
                k.dma("sp", ybt[:], ybv[:, :, bs], writes=[ybt])
                k.dma("sp", ut[:], uTv[:, :, bs], writes=[ut])
                k.op("pool", lambda: nc.gpsimd.tensor_tensor(out=yft[:], in0=yft[:], in1=ybt[:], op=ALU.add), reads=[yft, ybt], writes=[yft])
                for c in range(8):
                    k.op("dve", lambda: nc.vector.scalar_tensor_tensor(out=zt[:, c, :], in0=ut[:, c, :], scalar=dsk[:, c:c + 1], in1=yft[:, c, :], op0=ALU.mult, op1=ALU.add),
                         reads=[ut, dsk, yft], writes=[zt], partial=(c > 0))
                k.op("pool", lambda: nc.gpsimd.tensor_tensor(out=z2[:], in0=zt[:], in1=zt[:], op=ALU.mult), reads=[zt], writes=[z2])
                k.op("dve", lambda: nc.vector.tensor_scalar(out=z2[:], in0=z2[:], scalar1=0.0713548162726, scalar2=1.5957691216057308, op0=ALU.mult, op1=ALU.add), reads=[z2], writes=[z2])
                k.op("pool", lambda: nc.gpsimd.tensor_tensor(out=z2[:], in0=z2[:], in1=zt[:], op=ALU.mult), reads=[z2, zt], writes=[z2])
                k.op("act", lambda: nc.scalar.activation(out=z2[:], in_=z2[:], func=AF.Sigmoid), reads=[z2], writes=[z2])
                sm = ssm[b % 2]
                k.op("dve", lambda: nc.vector.tensor_tensor(out=sm[:], in0=z2[:], in1=zt[:], op=ALU.mult), reads=[z2, zt], writes=[sm])
                so = sot[b % 2]
                for j in range(8):
                    pv = PS[(2 * j) % 8]
                    pg = PS[(2 * j + 1) % 8]
                    for c in range(8):
                        k.op("pe", lambda: nc.tensor.matmul(pv[:], lhsT=wg[:, c, j * 128:(j + 1) * 128], rhs=sm[:, c, :], start=(c == 0), stop=(c == 7)), reads=[wg, sm], writes=[pv], partial=(c > 0))
                    for c in range(8):
                        k.op("pe", lambda: nc.tensor.matmul(pg[:], lhsT=wg[:, c, DSSM + j * 128:DSSM + (j + 1) * 128], rhs=sm[:, c, :], start=(c == 0), stop=(c == 7)), reads=[wg, sm], writes=[pg], partial=(c > 0))
                    sg = sgs[j % 2]
                    k.op("act", lambda: nc.scalar.activation(out=sg[:], in_=pg[:], func=AF.Sigmoid), reads=[pg], writes=[sg])
                    k.op("dve", lambda: nc.vector.tensor_tensor(out=so[:, j, :], in0=pv[:], in1=sg[:], op=ALU.mult), reads=[pv, sg], writes=[so], partial=(j > 0))
                k.dma("sp", soTv[:, :, bs], so[:], reads=[so])
        k.barrier()
        if stages <= 5:
            return nc

        for _sc in k.stage(6, skip):
            wba = k.sb("wba", [128, 16, D], BF16)
            wbs = k.sb("wbs", [128, 8, D], BF16)
            k.dma("pool", wba[:], P["w_br_attn"].rearrange("(c p) n -> p c n", p=128), writes=[wba])
            k.dma("pool", wbs[:], P["w_br_ssm"].rearrange("(c p) n -> p c n", p=128), writes=[wbs])
            aTv = attnT_d.rearrange("(c p) t -> p c t", p=128)
            soTv = soT_d.rearrange("(c p) t -> p c t", p=128)
            gav = gaT_d.rearrange("(c p) t -> p c t", p=128)
            gsv = gsT_d.rearrange("(c p) t -> p c t", p=128)
            mTv = mT_d.rearrange("(c p) t -> p c t", p=128)
            at = k.sb("at", [128, 16, 512], BF16)
            st = k.sb("st", [128, 8, 512], BF16)
            gat = k.sb("gat", [128, 16, 512], BF16)
            gst = k.sb("gst", [128, 16, 512], BF16)
            mt = [k.sb("mt", [128, 16, 512], BF16) for _ in range(2)]
            t1s = [k.sb("t1", [128, 512], F32) for _ in range(2)]
            t2s = [k.sb("t2", [128, 512], F32) for _ in range(2)]
            for b in range(NB):
                bs = slice(b * 512, (b + 1) * 512)
                k.dma("sp", at[:], aTv[:, :, bs], writes=[at])
                k.dma("sp", st[:], soTv[:, :, bs], writes=[st])
                k.dma("sp", gat[:], gav[:, :, bs], writes=[gat])
                k.dma("sp", gst[:], gsv[:, :, bs], writes=[gst])
                m = mt[b % 2]
                for dch in range(16):
                    pa = PS[(2 * dch) % 8]
                    pss = PS[(2 * dch + 1) % 8]
                    for c in range(16):
                        k.op("pe", lambda: nc.tensor.matmul(pa[:], lhsT=wba[:, c, dch * 128:(dch + 1) * 128], rhs=at[:, c, :], start=(c == 0), stop=(c == 15)), reads=[wba, at], writes=[pa], partial=(c > 0))
                    for c in range(8):
                        k.op("pe", lambda: nc.tensor.matmul(pss[:], lhsT=wbs[:, c, dch * 128:(dch + 1) * 128], rhs=st[:, c, :], start=(c == 0), stop=(c == 7)), reads=[wbs, st], writes=[pss], partial=(c > 0))
                    t1, t2 = t1s[dch % 2], t2s[dch % 2]
                    k.op("dve", lambda: nc.vector.tensor_tensor(out=t1[:], in0=pa[:], in1=gat[:, dch, :], op=ALU.mult), reads=[pa, gat], writes=[t1])
                    k.op("dve", lambda: nc.vector.tensor_tensor(out=t2[:], in0=pss[:], in1=gst[:, dch, :], op=ALU.mult), reads=[pss, gst], writes=[t2])
                    k.op("pool", lambda: nc.gpsimd.tensor_tensor(out=m[:, dch, :], in0=t1[:], in1=t2[:], op=ALU.add), reads=[t1, t2], writes=[m], partial=(dch > 0))
                k.dma("sp", mTv[:, :, bs], m[:], reads=[m])
        k.barrier()
        if stages <= 6:
            return nc

        AFF = k.sb("AFF", [128, NE, NT], F32)
        YT = k.wrap(y_out, "y_out")
        for _sc in k.stage(7, skip):
            wo = k.sb("wo", [128, 16, D], BF16)
            k.dma("pool", wo[:], P["w_out"].rearrange("(c p) n -> p c n", p=128), writes=[wo])
            wr = k.sb("wr", [128, 16, NE], F32)
            k.dma("sp", wr[:], P["w_router"].rearrange("(c p) e -> p c e", p=128), writes=[wr])
            gf = k.sb("gf", [128, D], F32)
            k.dma("sp", gf[:], P["g_ffn"][0:1, :].to_broadcast([128, D]), writes=[gf])
            valid = k.sb("valid", [128, NT], F32)
            vm1 = k.sb("vm1", [128, NT], F32)
            k.dma("sp", valid[:], C["valid"][:, :], writes=[valid])
            k.op("dve", lambda: nc.vector.tensor_scalar(out=vm1[:], in0=valid[:], scalar1=-1.0, scalar2=None, op0=ALU.add), reads=[valid], writes=[vm1])
            mTv = mT_d.rearrange("(c p) t -> p c t", p=128)
            mts = [k.sb("mtb", [128, 16, 512], BF16) for _ in range(2)]
            xts = [k.sb("xt", [128, D], F32) for _ in range(2)]
            x1s = [k.sb("x1", [128, D], F32) for _ in range(2)]
            xfs = [k.sb("xn2f", [128, D], F32) for _ in range(2)]
            xbs2 = [k.sb("xn2b", [128, D], BF16) for _ in range(2)]
            junk = k.sb("junk2", [128, D], BF16)
            xT2 = k.sb("xT2", [128, 16, 128], F32)
            ssq = [k.sb("ssq2", [128, 1], F32) for _ in range(2)]
            std = [k.sb("std2", [128, 1], F32) for _ in range(2)]
            rstd = [k.sb("rstd2", [128, 1], F32) for _ in range(2)]
            mx = [k.sb("mx", [128, 1], F32) for _ in range(2)]
            sme = [k.sb("sme", [128, 1], F32) for _ in range(2)]
            ex = [k.sb("ex", [128, NE], F32) for _ in range(2)]
            for b in range(NB):
                mtb = mts[b % 2]
                k.dma("sp", mtb[:], mTv[:, :, b * 512:(b + 1) * 512], writes=[mtb])
                for sub in range(4):
                    i = b * 4 + sub
                    a = i % 2
                    xt, x1, xf, xb2 = xts[a], x1s[a], xfs[a], xbs2[a]
                    k.dma("sp", xt[:], x_in[i * 128:(i + 1) * 128, :], writes=[xt])
                    for dq in range(4):
                        pt = PS[dq]
                        for c in range(16):
                            k.op("pe", lambda: nc.tensor.matmul(pt[:], lhsT=mtb[:, c, sub * 128:(sub + 1) * 128], rhs=wo[:, c, dq * 512:(dq + 1) * 512], start=(c == 0), stop=(c == 15)),
                                 reads=[mtb, wo], writes=[pt], partial=(c > 0))
                        k.op("dve", lambda: nc.vector.tensor_tensor(out=x1[:, dq * 512:(dq + 1) * 512], in0=pt[:], in1=xt[:, dq * 512:(dq + 1) * 512], op=ALU.add), reads=[pt, xt], writes=[x1], partial=(dq > 0))
                    k.dma("sp", y_out[i * 128:(i + 1) * 128, :], x1[:], reads=[x1], writes=[YT], partial=True)
                    k.op("act", lambda: nc.scalar.activation(out=junk[:], in_=x1[:], func=AF.Square, accum_out=ssq[a][:]), reads=[x1], writes=[junk, ssq[a]])
                    k.op("act", lambda: nc.scalar.activation(out=std[a][:], in_=ssq[a][:], func=AF.Sqrt, bias=eps_t[:], scale=1.0 / D), reads=[ssq[a], eps_t], writes=[std[a]])
                    k.op("dve", lambda: nc.vector.reciprocal(out=rstd[a][:], in_=std[a][:]), reads=[std[a]], writes=[rstd[a]])
                    k.op("dve", lambda: nc.vector.scalar_tensor_tensor(out=xf[:], in0=x1[:], scalar=rstd[a][:, 0:1], in1=gf[:], op0=ALU.mult, op1=ALU.mult), reads=[x1, rstd[a], gf], writes=[xf])
                    k.op("pool", lambda: nc.gpsimd.tensor_copy(out=xb2[:], in_=xf[:]), reads=[xf], writes=[xb2])
                    k.dma("sp", xn2_d[i * 128:(i + 1) * 128, :], xb2[:], reads=[xb2])
                    for q4 in range(4):
                        ptt = PS[4 + q4 % 2]
                        for j in range(4):
                            c = q4 * 4 + j
                            k.op("pe", lambda: nc.tensor.transpose(out=ptt[:, j * 128:(j + 1) * 128], in_=xf[:, c * 128:(c + 1) * 128], identity=ident_f[:]), reads=[xf, ident_f], writes=[ptt], partial=(j > 0))
                        if q4 % 2 == 0:
                            k.op("act", lambda: nc.scalar.copy(out=xT2[:, q4 * 4:(q4 + 1) * 4, :], in_=ptt[:].rearrange("p (c t) -> p c t", c=4)), reads=[ptt], writes=[xT2], partial=(q4 > 0))
                        else:
                            k.op("dve", lambda: nc.vector.tensor_copy(out=xT2[:, q4 * 4:(q4 + 1) * 4, :], in_=ptt[:].rearrange("p (c t) -> p c t", c=4)), reads=[ptt], writes=[xT2], partial=True)
                    pl = PS[6 + (i % 2)]
                    for c in range(16):
                        k.op("pe", lambda: nc.tensor.matmul(pl[:, 0:NE], lhsT=xT2[:, c, :], rhs=wr[:, c, :], start=(c == 0), stop=(c == 15)), reads=[xT2, wr], writes=[pl], partial=(c > 0))
                    k.op("dve", lambda: nc.vector.tensor_reduce(out=mx[a][:], in_=pl[:, 0:NE], axis=AX.X, op=ALU.max), reads=[pl], writes=[mx[a]])
                    k.op("dve", lambda: nc.vector.tensor_scalar(out=mx[a][:], in0=mx[a][:], scalar1=-1.0, scalar2=None, op0=ALU.mult), reads=[mx[a]], writes=[mx[a]])
                    k.op("act", lambda: nc.scalar.activation(out=ex[a][:], in_=pl[:, 0:NE], func=AF.Exp, bias=mx[a][:, 0:1], scale=1.0, accum_out=sme[a][:]), reads=[pl, mx[a]], writes=[ex[a], sme[a]])
                    k.op("dve", lambda: nc.vector.reciprocal(out=sme[a][:], in_=sme[a][:]), reads=[sme[a]], writes=[sme[a]])
                    k.op("dve", lambda: nc.vector.tensor_scalar(out=ex[a][:], in0=ex[a][:], scalar1=sme[a][:, 0:1], scalar2=valid[:, i:i + 1], op0=ALU.mult, op1=ALU.mult), reads=[ex[a], sme[a], valid], writes=[ex[a]])
                    k.op("dve", lambda: nc.vector.tensor_scalar(out=AFF[:, :, i], in0=ex[a][:], scalar1=vm1[:, i:i + 1], scalar2=None, op0=ALU.add), reads=[ex[a], vm1], writes=[AFF], partial=True)
            if debug:
                k.dma("sp", aff_d[:, :], AFF[:].rearrange("p e t -> p (e t)"), reads=[AFF])
        k.barrier()
        if stages <= 7:
            return nc

        BIG = float(1 << 22)
        NITER = 40
        for _sc in k.stage(8, skip):
            ones_f = k.sb("ones_f", [128, 128], F32)
            tri_b = k.sb("tri_b", [128, 128], BF16)
            tri_f = k.sb("tri_f", [128, 128], F32)
            k.op("dve", lambda: nc.vector.memset(ones_f[:], 1.0), writes=[ones_f])
            k.dma("sp", tri_f[:], C["tri"][:, :], writes=[tri_f])
            k.op("dve", lambda: nc.vector.tensor_copy(out=tri_b[:], in_=tri_f[:]), reads=[tri_f], writes=[tri_b])
            kcap = k.sb("kcap", [128, NE], F32)
            k.dma("sp", kcap[:], C["kcap"][:, :], writes=[kcap])
            tokid = k.sb("tokid", [128, NT], F32)
            k.dma("sp", tokid[:], C["tokid"][:, :], writes=[tokid])
            lo = k.sb("lo", [128, NE], F32)
            hi = k.sb("hi", [128, NE], F32)
            mid = k.sb("mid", [128, NE], F32)
            cnt = k.sb("cnt", [128, NE], F32)
            ge = k.sb("ge", [128, NE], F32)
            dl = k.sb("dl", [128, NE], F32)
            cmpj = k.sb("cmpj", [128, NE, NT], F32)
            k.op("dve", lambda: nc.vector.memset(lo[:], 0.0), writes=[lo])
            k.op("dve", lambda: nc.vector.memset(hi[:], 2.0), writes=[hi])
            pc = PS[0]
            for it in range(NITER):
                k.op("dve", lambda: nc.vector.tensor_tensor(out=mid[:], in0=lo[:], in1=hi[:], op=ALU.add), reads=[lo, hi], writes=[mid])
                k.op("dve", lambda: nc.vector.tensor_scalar(out=mid[:], in0=mid[:], scalar1=0.5, scalar2=None, op0=ALU.mult), reads=[mid], writes=[mid])
                for e in range(NE):
                    k.op("dve", lambda: nc.vector.tensor_scalar(out=cmpj[:, e, :], in0=AFF[:, e, :], scalar1=mid[:, e:e + 1], scalar2=0.0, op0=ALU.is_ge, op1=ALU.add, accum_out=cnt[:, e:e + 1]),
                         reads=[AFF, mid], writes=[cmpj, cnt], partial=(e > 0))
                k.op("pe", lambda: nc.tensor.matmul(pc[:, 0:NE], lhsT=ones_f[:], rhs=cnt[:], start=True, stop=True), reads=[ones_f, cnt], writes=[pc])
                k.op("dve", lambda: nc.vector.tensor_tensor(out=ge[:], in0=pc[:, 0:NE], in1=kcap[:], op=ALU.is_ge), reads=[pc, kcap], writes=[ge])
                k.op("dve", lambda: nc.vector.tensor_tensor(out=dl[:], in0=mid[:], in1=lo[:], op=ALU.subtract), reads=[mid, lo], writes=[dl])
                k.op("dve", lambda: nc.vector.tensor_tensor(out=dl[:], in0=dl[:], in1=ge[:], op=ALU.mult), reads=[dl, ge], writes=[dl])
                k.op("dve", lambda: nc.vector.tensor_tensor(out=lo[:], in0=lo[:], in1=dl[:], op=ALU.add), reads=[lo, dl], writes=[lo])
                k.op("dve", lambda: nc.vector.tensor_tensor(out=dl[:], in0=hi[:], in1=mid[:], op=ALU.subtract), reads=[hi, mid], writes=[dl])
                k.op("dve", lambda: nc.vector.tensor_tensor(out=dl[:], in0=dl[:], in1=ge[:], op=ALU.mult), reads=[dl, ge], writes=[dl])
                k.op("dve", lambda: nc.vector.tensor_tensor(out=hi[:], in0=mid[:], in1=dl[:], op=ALU.add), reads=[mid, dl], writes=[hi])
            maskb = k.sb("maskb", [128, NE, NT], BF16)
            maskf = k.sb("maskf", [128, NE, NT], F32)
            for e in range(NE):
                k.op("dve", lambda: nc.vector.tensor_scalar(out=maskf[:, e, :], in0=AFF[:, e, :], scalar1=lo[:, e:e + 1], scalar2=None, op0=ALU.is_ge), reads=[AFF, lo], writes=[maskf], partial=(e > 0))
            k.op("pool", lambda: nc.gpsimd.tensor_copy(out=maskb[:], in_=maskf[:]), reads=[maskf], writes=[maskb])
            posw = k.sb("posw", [128, NE, NT], F32)
            tot = k.sb("tot", [128, NE, NT], F32)
            mflat = maskb[:].rearrange("p e t -> p (e t)")
            pflat = posw[:].rearrange("p e t -> p (e t)")
            tflat = tot[:].rearrange("p e t -> p (e t)")
            W = NE * NT
            for c0 in range(0, W, 512):
                c1 = min(W, c0 + 512)
                p1 = PS[1 + (c0 // 512) % 2]
                p2 = PS[3 + (c0 // 512) % 2]
                k.op("pe", lambda: nc.tensor.matmul(p1[:, 0:c1 - c0], lhsT=tri_b[:], rhs=mflat[:, c0:c1], start=True, stop=True), reads=[tri_b, maskb], writes=[p1])
                k.op("pe", lambda: nc.tensor.matmul(p2[:, 0:c1 - c0], lhsT=ones_b[:], rhs=mflat[:, c0:c1], start=True, stop=True), reads=[ones_b, maskb], writes=[p2])
                k.op("act", lambda: nc.scalar.copy(out=pflat[:, c0:c1], in_=p1[:, 0:c1 - c0]), reads=[p1], writes=[posw], partial=(c0 > 0))
                k.op("dve", lambda: nc.vector.tensor_copy(out=tflat[:, c0:c1], in_=p2[:, 0:c1 - c0]), reads=[p2], writes=[tot], partial=(c0 > 0))
            cum = k.sb("cum", [128, NE, NT], F32)
            for e in range(NE):
                k.op("dve", lambda: nc.vector.tensor_tensor_scan(out=cum[:, e, :], data0=ones_f[:, 0:1].to_broadcast([128, NT]), data1=tot[:, e, :], initial=0.0, op0=ALU.mult, op1=ALU.add),
                     reads=[ones_f, tot], writes=[cum], partial=(e > 0))
            k.op("pool", lambda: nc.gpsimd.tensor_tensor(out=cum[:], in0=cum[:], in1=tot[:], op=ALU.subtract), reads=[cum, tot], writes=[cum])
            k.op("pool", lambda: nc.gpsimd.tensor_tensor(out=posw[:], in0=posw[:], in1=cum[:], op=ALU.add), reads=[posw, cum], writes=[posw])
            for e in range(NE):
                k.op("dve", lambda: nc.vector.tensor_scalar(out=posw[:, e, :], in0=posw[:, e, :], scalar1=float(e * CAP) - BIG, scalar2=None, op0=ALU.add), reads=[posw], writes=[posw], partial=(e > 0))
            k.op("pool", lambda: nc.gpsimd.tensor_tensor(out=posw[:], in0=posw[:], in1=maskf[:], op=ALU.mult), reads=[posw, maskf], writes=[posw])
            k.op("dve", lambda: nc.vector.tensor_scalar(out=posw[:], in0=posw[:], scalar1=BIG, scalar2=None, op0=ALU.add), reads=[posw], writes=[posw])
            IDX = k.sb("IDX", [128, NE, NT], I32)
            k.op("dve", lambda: nc.vector.tensor_copy(out=IDX[:], in_=posw[:]), reads=[posw], writes=[IDX])
            PAIR = k.sb("PAIR", [128, NE, NT, 2], F32)
            for e in range(NE):
                k.op("pool", lambda: nc.gpsimd.tensor_copy(out=PAIR[:, e, :, 0], in_=tokid[:]), reads=[tokid], writes=[PAIR], partial=(e > 0))
            k.op("dve", lambda: nc.vector.tensor_copy(out=PAIR[:, :, :, 1], in_=AFF[:]), reads=[AFF], writes=[PAIR], partial=True)
            ini = k.sb("ini", [128, (NE * CAP) // 128, 2], F32)
            sid = k.sb("sid", [128, (NE * CAP) // 128], F32)
            k.dma("sp", sid[:], C["slotid"][:, :], writes=[sid])
            k.op("dve", lambda: nc.vector.memset(ini[:], 0.0), writes=[ini])
            k.op("dve", lambda: nc.vector.tensor_copy(out=ini[:, :, 0], in_=sid[:]), reads=[sid], writes=[ini])
            LST = k.wrap(lst_d, "lst_d")
            r_lst = nc.gpsimd.alloc_register("r_lst")
            nc.gpsimd.reg_mov(r_lst, NE * CAP - 1)
            k.dma("sp", lst_d.rearrange("(p r) c -> p r c", p=128), ini[:], reads=[ini], writes=[LST])
            for e in range(NE):
                for i in range(NT):
                    k.idma(lst_d[:, :], bass.IndirectOffsetOnAxis(ap=IDX[:, e, i:i + 1], axis=0), PAIR[:, e, i, :], None,
                           reads=[IDX, PAIR], writes=[LST], partial=True, bounds_check=r_lst, oob_is_err=False)
        k.barrier()
        if stages <= 8:
            return nc

        for _sc in k.stage(9, skip):
            CTL = CAP // 128
            SB = min(512, CAP)
            TPS = SB // 128
            NSB = CAP // SB
            wd = k.sb("wd", [128, 16, D], BF16)
            wgs = [k.sb("wgt", [128, 16, 256], BF16) for _ in range(2)]
            wus = [k.sb("wut", [128, 16, 256], BF16) for _ in range(2)]
            xeT = k.sb("xeT", [128, 16, SB], BF16)
            hT = k.sb("hT", [128, 16, SB], BF16)
            XE = [k.sb("XE", [128, D], BF16) for _ in range(2)]
            yes = [k.sb("ye", [128, D], F32) for _ in range(2)]
            sgm = [k.sb("sgm", [128, SB], F32) for _ in range(2)]
            lstt = k.sb("lstt", [128, CTL, 2], F32)
            idxi = k.sb("idxi", [128, CTL], I32)
            gts = k.sb("gts", [128, CTL], F32)
            for xe in XE:
                k.op("dve", lambda: nc.vector.memset(xe[:], 0.0), writes=[xe])
            fgc = 0
            for e in range(NE):
                k.dma("sp", lstt[:], lst_d[e * CAP:(e + 1) * CAP, :].rearrange("(t p) c -> p t c", p=128), writes=[lstt])
                k.op("dve", lambda: nc.vector.tensor_copy(out=idxi[:], in_=lstt[:, :, 0]), reads=[lstt], writes=[idxi])
                k.op("dve", lambda: nc.vector.tensor_copy(out=gts[:], in_=lstt[:, :, 1]), reads=[lstt], writes=[gts])
                k.dma("pool", wd[:], P["w_down"][e].rearrange("(c p) n -> p c n", p=128), writes=[wd])
                wgv = P["w_gate"][e].rearrange("(c p) n -> p c n", p=128)
                wuv = P["w_up"][e].rearrange("(c p) n -> p c n", p=128)
                for sbi in range(NSB):
                    for t in range(TPS):
                        tile = sbi * TPS + t
                        xe = XE[tile % 2]
                        k.idma(xe[:], None, xn2_d[:, :], bass.IndirectOffsetOnAxis(ap=idxi[:, tile:tile + 1], axis=0), reads=[idxi], writes=[xe], partial=False)
                        chk(91)
                        for hlf in range(2):
                            pst = PS[(2 * tile + hlf) % 4]
                            pv = pst.t[:].bitcast(BF16)
                            for j in range(8):
                                c = hlf * 8 + j
                                k.op("pe", lambda: nc.tensor.transpose(out=pv[:, j * 128:(j + 1) * 128], in_=xe[:, c * 128:(c + 1) * 128], identity=ident_b[:]),
                                     reads=[xe, ident_b], writes=[pst], partial=(j > 0))
                            dstv = xeT[:, hlf * 8:(hlf + 1) * 8, t * 128:(t + 1) * 128]
                            srcv = pv[:, 0:1024].rearrange("p (c t) -> p c t", c=8)
                            if hlf == 0:
                                k.op("act", lambda: nc.scalar.copy(out=dstv, in_=srcv), reads=[pst], writes=[xeT], partial=not (t == 0))
                            else:
                                k.op("dve", lambda: nc.vector.tensor_copy(out=dstv, in_=srcv), reads=[pst], writes=[xeT], partial=True)
                    for fg in range(8):
                        wgt, wut = wgs[fgc % 2], wus[fgc % 2]
                        fgc += 1
                        k.dma("pool", wgt[:], wgv[:, :, fg * 256:(fg + 1) * 256], writes=[wgt])
                        k.dma("pool", wut[:], wuv[:, :, fg * 256:(fg + 1) * 256], writes=[wut])
                        for fc in range(2):
                            fch = fg * 2 + fc
                            pg = PS[4 + 2 * (fch % 2)]
                            pu = PS[5 + 2 * (fch % 2)]
                            for c in range(16):
                                k.op("pe", lambda: nc.tensor.matmul(pg[:, 0:SB], lhsT=wgt[:, c, fc * 128:(fc + 1) * 128], rhs=xeT[:, c, :], start=(c == 0), stop=(c == 15)), reads=[wgt, xeT], writes=[pg], partial=(c > 0))
                            for c in range(16):
                                k.op("pe", lambda: nc.tensor.matmul(pu[:, 0:SB], lhsT=wut[:, c, fc * 128:(fc + 1) * 128], rhs=xeT[:, c, :], start=(c == 0), stop=(c == 15)), reads=[wut, xeT], writes=[pu], partial=(c > 0))
                            sg = sgm[fch % 2]
                            k.op("act", lambda: nc.scalar.activation(out=sg[:], in_=pg[:, 0:SB], func=AF.Silu), reads=[pg], writes=[sg])
                            k.op("dve", lambda: nc.vector.tensor_tensor(out=hT[:, fch, :], in0=pu[:, 0:SB], in1=sg[:], op=ALU.mult), reads=[pu, sg], writes=[hT], partial=(fch > 0))
                    for t in range(TPS):
                        tile = sbi * TPS + t
                        ye = yes[tile % 2]
                        for dq in range(4):
                            pt = PS[dq]
                            for fch in range(16):
                                k.op("pe", lambda: nc.tensor.matmul(pt[:], lhsT=hT[:, fch, t * 128:(t + 1) * 128], rhs=wd[:, fch, dq * 512:(dq + 1) * 512], start=(fch == 0), stop=(fch == 15)),
                                     reads=[hT, wd], writes=[pt], partial=(fch > 0))
                            k.op("act", lambda: nc.scalar.activation(out=ye[:, dq * 512:(dq + 1) * 512], in_=pt[:], func=AF.Copy, scale=gts[:, tile:tile + 1]), reads=[pt, gts], writes=[ye], partial=(dq > 0))
                        chk(92)
                        k.idma(y_out[:, :], bass.IndirectOffsetOnAxis(ap=idxi[:, tile:tile + 1], axis=0), ye[:], None, reads=[ye, idxi], writes=[YT], partial=False,
                               compute_op=ALU.add)
                        chk(93)
        k.finish()
    return nc


_PROG = {}


def kernel(**inputs):
    cfg = FULL_CFG
    if "nc" not in _PROG:
        _PROG["nc"] = build_program(cfg)
    nc = _PROG["nc"]
    params = {}
    for n, shp in PARAM_SHAPES.items():
        a = np.asarray(inputs[n], dtype=np.float32)
        params[n] = np.ascontiguousarray(a[0].reshape(shp))
    xp = np.asarray(inputs["x_prompt"], dtype=np.float32)
    xs = np.asarray(inputs["x_sample"], dtype=np.float32)
    NTC = cfg["NTC"]
    x0 = np.ascontiguousarray(xp.reshape(-1, D))
    assert x0.shape[0] == NTC
    x1 = np.zeros((NTC, D), np.float32)
    ns = xs.shape[0] * xs.shape[1]
    x1[:ns] = xs.reshape(-1, D)
    c0 = host_constants(cfg, [xp.shape[1]] * xp.shape[0], max(1, min(x0.shape[0], 2 * x0.shape[0] // NE)))
    c1 = host_constants(cfg, [xs.shape[1]] * xs.shape[0], max(1, min(ns, 2 * ns // NE)))
    m0 = dict(params)
    m0.update(c0)
    m0["x"] = x0
    m1 = dict(params)
    m1.update(c1)
    m1["x"] = x1
    in_maps = [m0, m1]
    res = run_bass_kernel_spmd(nc, in_maps, core_ids=list(range(len(in_maps))))
    y0 = np.asarray(res.results[0]["y"], dtype=np.float32).reshape(xp.shape)
    y1 = np.asarray(res.results[1]["y"], dtype=np.float32)[:ns].reshape(xs.shape)
    return (y0, y1)
```
